# Optimizing a Trainium2 kernel written in Bass

```python
import jax, jax.numpy as jnp
from jax import lax
import numpy as np

D_MODEL = 1024
BATCH = 32
SEQ = 2048
DEPTH = 1
DEC_BATCH = 4
DEC_SEQ = 4096
PAST_LEN = 128

CONV_W = D_MODEL // 2
POOL_W = D_MODEL // 2
N_POOL_GROUPS = 4
POOL_GC = POOL_W // N_POOL_GROUPS
POOL_WINDOWS = (2, 4, 8, 16)
KERNEL_SIZE = 31
IN_COLS = 2 * CONV_W + POOL_W
N_EXPERTS = 16
CAPACITY_FACTOR = 2
D_EXPERT = 2 * D_MODEL
PLE_DIM = 256
EPS = 1e-6

kernel_name = "hybrid_conv_pool_ec_encoder"


def rmsnorm(x, g):
    xf = x.astype(jnp.float32)
    y = xf * lax.rsqrt(jnp.mean(xf * xf, axis=-1, keepdims=True) + EPS)
    return (y * g.astype(jnp.float32)).astype(x.dtype)


def layernorm(x, g, b):
    xf = x.astype(jnp.float32)
    mu = jnp.mean(xf, axis=-1, keepdims=True)
    var = jnp.mean(jnp.square(xf - mu), axis=-1, keepdims=True)
    y = (xf - mu) * lax.rsqrt(var + EPS)
    return (y * g.astype(jnp.float32) + b.astype(jnp.float32)).astype(x.dtype)


def conv_module(za, zg, conv_k, conv_b, ln_g, ln_b):
    u = za * jax.nn.sigmoid(zg)
    pad = KERNEL_SIZE // 2
    u = lax.conv_general_dilated(u, conv_k.astype(u.dtype), window_strides=(1,),
                                 padding=[(pad, pad)],
                                 dimension_numbers=("NWC", "WIO", "NWC"),
                                 feature_group_count=CONV_W)
    u = u + conv_b
    u = layernorm(u, ln_g, ln_b)
    return jax.nn.silu(u)


def centred_mean_minus_self(u, w):
    L = u.shape[1]
    uf = u.astype(jnp.float32)
    c = jnp.cumsum(uf, axis=1)
    c = jnp.concatenate([jnp.zeros_like(c[:, :1]), c], axis=1)
    t = jnp.arange(L)
    lo = jnp.maximum(t - w // 2, 0)
    hi = jnp.minimum(t + w // 2 - 1, L - 1)
    s = jnp.take(c, hi + 1, axis=1) - jnp.take(c, lo, axis=1)
    cnt = (hi - lo + 1).astype(jnp.float32)[None, :, None]
    return (s / cnt - uf).astype(u.dtype)


def pool_module(zp, pool_w, pool_scale):
    B, L, _ = zp.shape
    zg = zp.reshape(B, L, N_POOL_GROUPS, POOL_GC)
    pooled = jnp.stack([centred_mean_minus_self(zg[:, :, g], POOL_WINDOWS[g])
                        for g in range(N_POOL_GROUPS)], axis=2)
    y = jnp.einsum("blgc,gcd->blgd", pooled, pool_w) * pool_scale
    return y.reshape(B, L, POOL_W)


def expert_choice_moe(h, w_router, w_gate, w_up, w_down):
    B, L, D = h.shape
    n_tok = B * L
    t = h.reshape(n_tok, D)
    probs = jax.nn.softmax((t @ w_router).astype(jnp.float32), axis=-1)
    cap = CAPACITY_FACTOR * n_tok // N_EXPERTS
    gates, idx = lax.top_k(probs.T, cap)
    xe = jnp.take(t, idx, axis=0)
    a = jnp.einsum("ecd,edf->ecf", xe, w_gate)
    b = jnp.einsum("ecd,edf->ecf", xe, w_up)
    ye = jnp.einsum("ecf,efd->ecd", jax.nn.silu(a) * b, w_down)
    ye = ye * gates[..., None].astype(ye.dtype)
    out = jnp.zeros_like(t).at[idx.reshape(-1)].add(ye.reshape(-1, D))
    return out.reshape(B, L, D)


def run_trunk(x, p, norm1_g, w_in, conv_k, conv_b, conv_ln_g, conv_ln_b, pool_w, pool_scale,
              w_out, norm2_g, w_router, w_gate, w_up, w_down, w_ple, w_pg, b_pg, final_g):
    for i in range(DEPTH):
        h = rmsnorm(x, norm1_g[i])
        z = h @ w_in[i]
        za = z[..., :CONV_W]
        zg = z[..., CONV_W:2 * CONV_W]
        zp = z[..., 2 * CONV_W:]
        yc = conv_module(za, zg, conv_k[i], conv_b[i], conv_ln_g[i], conv_ln_b[i])
        yp = pool_module(zp, pool_w[i], pool_scale[i])
        x = x + jnp.concatenate([yc, yp], axis=-1) @ w_out[i]
        x = x + expert_choice_moe(rmsnorm(x, norm2_g[i]), w_router[i], w_gate[i], w_up[i], w_down[i])
        gate = jax.nn.sigmoid(x @ w_pg[i] + b_pg[i])
        x = x + gate * (p[i] @ w_ple[i])
    return rmsnorm(x, final_g)


def setup_inputs(seed: int = 0) -> dict:
    key = jax.random.key(seed)
    ks = jax.random.split(key, 24)
    f = jnp.float32
    nrm = lambda k, shape, scale: jax.random.normal(k, shape, f) * scale
    return {
        "x_prompt": nrm(ks[0], (BATCH, SEQ, D_MODEL), 1.0),
        "x_sample": nrm(ks[1], (DEC_BATCH, DEC_SEQ, D_MODEL), 1.0),
        "p_prompt": nrm(ks[2], (DEPTH, BATCH, SEQ, PLE_DIM), 1.0),
        "p_sample": nrm(ks[3], (DEPTH, DEC_BATCH, DEC_SEQ, PLE_DIM), 1.0),
        "norm1_g": 1.0 + nrm(ks[4], (DEPTH, D_MODEL), 0.02),
        "w_in": nrm(ks[5], (DEPTH, D_MODEL, IN_COLS), D_MODEL ** -0.5),
        "conv_k": nrm(ks[6], (DEPTH, KERNEL_SIZE, 1, CONV_W), KERNEL_SIZE ** -0.5),
        "conv_b": nrm(ks[7], (DEPTH, CONV_W), 0.02),
        "conv_ln_g": 1.0 + nrm(ks[8], (DEPTH, CONV_W), 0.02),
        "conv_ln_b": nrm(ks[9], (DEPTH, CONV_W), 0.02),
        "pool_w": nrm(ks[10], (DEPTH, N_POOL_GROUPS, POOL_GC, POOL_GC), POOL_GC ** -0.5),
        "pool_scale": 1.0 + nrm(ks[11], (DEPTH, N_POOL_GROUPS, POOL_GC), 0.1),
        "w_out": nrm(ks[12], (DEPTH, D_MODEL, D_MODEL), D_MODEL ** -0.5),
        "norm2_g": 1.0 + nrm(ks[13], (DEPTH, D_MODEL), 0.02),
        "w_router": nrm(ks[14], (DEPTH, D_MODEL, N_EXPERTS), D_MODEL ** -0.5),
        "w_gate": nrm(ks[15], (DEPTH, N_EXPERTS, D_MODEL, D_EXPERT), D_MODEL ** -0.5),
        "w_up": nrm(ks[16], (DEPTH, N_EXPERTS, D_MODEL, D_EXPERT), D_MODEL ** -0.5),
        "w_down": nrm(ks[17], (DEPTH, N_EXPERTS, D_EXPERT, D_MODEL), D_EXPERT ** -0.5),
        "w_ple": nrm(ks[18], (DEPTH, PLE_DIM, D_MODEL), PLE_DIM ** -0.5),
        "w_pg": nrm(ks[19], (DEPTH, D_MODEL, D_MODEL), D_MODEL ** -0.5),
        "b_pg": nrm(ks[20], (DEPTH, D_MODEL), 0.02),
        "final_g": 1.0 + nrm(ks[21], (D_MODEL,), 0.02),
    }


def reference(x_prompt, x_sample, p_prompt, p_sample, norm1_g, w_in, conv_k, conv_b, conv_ln_g,
              conv_ln_b, pool_w, pool_scale, w_out, norm2_g, w_router, w_gate, w_up, w_down,
              w_ple, w_pg, b_pg, final_g):
    y_prompt = run_trunk(x_prompt, p_prompt, norm1_g, w_in, conv_k, conv_b, conv_ln_g, conv_ln_b,
                         pool_w, pool_scale, w_out, norm2_g, w_router, w_gate, w_up, w_down,
                         w_ple, w_pg, b_pg, final_g)
    y_sample = run_trunk(x_sample, p_sample, norm1_g, w_in, conv_k, conv_b, conv_ln_g, conv_ln_b,
                         pool_w, pool_scale, w_out, norm2_g, w_router, w_gate, w_up, w_down,
                         w_ple, w_pg, b_pg, final_g)
    return (y_prompt, y_sample)
```

```python
from contextlib import ExitStack
import numpy as np
import ml_dtypes
import concourse.bass as bass
import concourse.mybir as mybir
from concourse.bass_utils import run_bass_kernel_spmd

F32 = mybir.dt.float32
BF16 = mybir.dt.bfloat16
I32 = mybir.dt.int32
AF = mybir.ActivationFunctionType
ALU = mybir.AluOpType
AX = mybir.AxisListType

NCORES = 8
D = 1024
SEQ = 2048
NPSEQ = 4
SEXT = 2048 + 256
TLOC = 10240
NT = 80
NE = 16
EPS = 1e-6
POOL_WINDOWS = (2, 4, 8, 16)
KS = 31
PADL = 16


class Buf:
    __slots__ = ("name", "last_w", "readers", "sem", "cnt")

    def __init__(self, name):
        self.name = name
        self.last_w = None
        self.readers = {}
        self.sem = None
        self.cnt = 0


class Prog:
    ENGS = ("pe", "act", "dve", "pool", "sp")

    def __init__(self, nc, es):
        self.nc = nc
        self.es = es
        self.streams = {e: [] for e in self.ENGS}
        self._regs = {}
        self.cnt = {e: 0 for e in self.ENGS}
        self.waited = {e: {} for e in self.ENGS}
        self.sems = {}
        for e in ("pe", "act", "dve", "pool"):
            self.sems[e] = es.enter_context(nc.semaphore("c_" + e))
        self.nbuf = 0
        self.dma_bufs = []
        self._regs = {}

    def reg(self, eng, val):
        key = (id(eng), val)
        if key not in self._regs:
            self._regs[key] = eng.to_reg(val)
        return self._regs[key]

    def barrier(self):
        toks = [(e, self.cnt[e]) for e in ("pe", "act", "dve", "pool") if self.cnt[e] > 0]
        toks += [(b, b.cnt) for b in self.dma_bufs if b.cnt > 0]
        for eng in self.ENGS:
            w = self.waited[eng]
            waits = []
            for (k, v) in toks:
                if k == eng or w.get(k, 0) >= v:
                    continue
                w[k] = v
                waits.append((k, v))
            self.streams[eng].append((waits, None, None))

    def buf(self, name=None):
        self.nbuf += 1
        return Buf(name or "b%d" % self.nbuf)

    def _sem_of(self, key):
        if isinstance(key, str):
            return self.sems[key]
        if key.sem is None:
            key.sem = self.es.enter_context(self.nc.semaphore("d_" + key.name))
            self.dma_bufs.append(key)
        return key.sem

    def op(self, eng, emit, reads=(), writes=(), dma=None, self_sync=True, skip_waw=False):
        deps = []
        raw = set()
        for b in reads:
            if b.last_w is not None:
                deps.append(b.last_w)
                raw.add(b.last_w)
        for b in writes:
            if b.last_w is not None and not skip_waw:
                deps.append(b.last_w)
            deps.extend(b.readers.items())
        if dma is not None:
            dma.cnt += 16
            tok = (dma, dma.cnt)
            self._sem_of(dma)
        else:
            self.cnt[eng] += 1
            tok = (eng, self.cnt[eng])
        waits = []
        w = self.waited[eng]
        for (k, v) in deps:
            if k == "pe" and eng == "pe":
                continue
            if k == eng and not self_sync and (k, v) not in raw:
                continue
            if w.get(k, 0) >= v:
                continue
            w[k] = v
            waits.append((k, v))
        self.streams[eng].append((waits, emit, tok))
        for b in reads:
            if b.readers.get(tok[0], 0) < tok[1]:
                b.readers[tok[0]] = tok[1]
        for b in writes:
            b.last_w = tok
            if not skip_waw:
                b.readers = {}
        return tok

    def final_wait(self, eng, bufs):
        waits = []
        for b in bufs:
            if b.last_w is not None:
                waits.append(b.last_w)
            waits.extend(b.readers.items())
        self.streams[eng].append((waits, None, None))

    def emit(self):
        nc = self.nc
        handles = {"pe": "tensor", "act": "scalar", "dve": "vector", "pool": "gpsimd", "sp": "sync"}
        with nc.Block() as block:
            for e in self.ENGS:
                stream = self.streams[e]
                if not stream:
                    continue

                def body(eng, stream=stream):
                    for (waits, emit, tok) in stream:
                        for (k, v) in waits:
                            eng.wait_ge(self._sem_of(k), v)
                        if emit is None:
                            continue
                        inst = emit(eng)
                        if isinstance(tok[0], str):
                            inst.then_inc(self.sems[tok[0]], 1)
                        else:
                            inst.then_inc(tok[0].sem, 16)

                getattr(block, handles[e])(body)
        self.streams = {e: [] for e in self.ENGS}
        self._regs = {}


def _sb(nc, es, name, shape, dt):
    return es.enter_context(nc.sbuf_tensor(name, list(shape), dt))


def build_phase_a(debug=False):
    nc = bass.Bass("TRN2", target_bir_lowering=False)
    es = ExitStack()
    P = Prog(nc, es)

    def din(name, shape, dt=F32):
        return nc.dram_tensor(name, list(shape), dt, kind="ExternalInput").ap()

    xp = din("xp", [NPSEQ * SEQ, D])
    xs = din("xs", [SEXT, D])
    w_in = din("w_in", [D, 1536])
    w_out = din("w_out", [D, D])
    w_router = din("w_router", [D, NE])
    convk = din("convk", [128, 4, KS])
    pvec = din("pvec", [128, 24])
    g2b = din("g2b", [128, D])
    pool_w = din("pool_w", [4, 128, 128])
    pedge = din("pedge", [128, 2, 4, 2, 8])
    x1_o = nc.dram_tensor("x1", [TLOC, D], F32, kind="ExternalOutput").ap()
    h2_o = nc.dram_tensor("h2", [TLOC, D], BF16, kind="ExternalOutput").ap()
    pr_o = nc.dram_tensor("probs", [128, NT, NE], F32, kind="ExternalOutput").ap()

    sb = lambda name, shape, dt: _sb(nc, es, name, shape, dt)
    ident_f = sb("ident_f", [128, 128], F32)
    ident = sb("ident", [128, 128], BF16)
    tapm = sb("tapm", [128, 4, 128], BF16)
    onesm = sb("onesm", [128, 128], BF16)
    win_b = sb("win_b", [128, 8, 1536], BF16)
    wout_b = sb("wout_b", [128, 8, D], BF16)
    wr_b = sb("wr_b", [128, 8, NE], BF16)
    wr_f = sb("wr_f", [128, 8, NE], F32)
    dconv = sb("dconv", [128, 4, KS, 128], BF16)
    convk_s = sb("convk_s", [128, 4, KS], F32)
    pvec_s = sb("pvec_s", [128, 24], F32)
    g2b_s = sb("g2b_s", [128, D], F32)
    poolw_f = sb("poolw_f", [128, 4, 128], F32)
    poolw_b = sb("poolw_b", [128, 4, 128], BF16)
    pedge_s = sb("pedge_s", [128, 2, 4, 2, 8], F32)
    epsb = sb("epsb", [128, 1], F32)
    LB = PADL + SEXT + PADL
    xt = [sb("xt%d" % i, [128, D], F32) for i in range(2)]
    xr = [sb("xr%d" % i, [128, D], F32) for i in range(2)]
    hb = [sb("hb%d" % i, [128, D], BF16) for i in range(2)]
    junk = sb("junk", [128, D], BF16)
    hT = [sb("hT%d" % i, [128, 8, 512], BF16) for i in range(2)]
    ubuf = sb("ubuf", [128, 4, LB], BF16)
    zpbuf = sb("zpbuf", [128, 4, LB], BF16)
    sig = [sb("sig%d" % i, [128, 512], F32) for i in range(2)]
    vb = sb("vb", [128, 4, 512], BF16)
    sq = sb("sq", [128, 4, 512], BF16)
    ycT = sb("ycT", [128, 4, 512], BF16)
    ypT = sb("ypT", [128, 4, 512], BF16)
    msq = sb("msq", [128, 512], F32)
    lvar = sb("lvar", [128, 512], F32)
    lrstd = sb("lrstd", [128, 512], F32)
    dtmp = [sb("dtmp%d" % i, [128, 512], F32) for i in range(2)]
    pooled = [sb("pooled%d" % i, [128, 512], BF16) for i in range(2)]
    x1t = [sb("x1t%d" % i, [128, D], F32) for i in range(1)]
    h2t = [sb("h2t%d" % i, [128, D], BF16) for i in range(2)]
    h2T = [sb("h2T%d" % i, [128, 8, 128], BF16) for i in range(2)]
    stat = sb("stat", [128, 8, 8], F32)
    probs_s = sb("probs_s", [128, NT, NE], F32)
    esm = sb("esm", [128, 2, NE], F32)
    ps = [es.enter_context(nc.psum_tensor("ps%d" % i, [128, 512], F32)) for i in range(8)]
    psB = [P.buf("ps%d" % i) for i in range(8)]

    B = {}

    def bf(name):
        if name not in B:
            B[name] = P.buf(name)
        return B[name]

    def dma(eng, out, in_, reads, writes, dbuf):
        return P.op(eng, lambda e: e.dma_start(out=out, in_=in_), reads=reads, writes=writes, dma=dbuf)

    dma("sp", convk_s[:], convk, [], [bf("convk_s")], bf("convk_s"))
    dma("sp", pvec_s[:], pvec, [], [bf("pvec_s")], bf("pvec_s"))
    dma("sp", g2b_s[:], g2b, [], [bf("g2b_s")], bf("g2b_s"))
    dma("sp", poolw_f[:], pool_w.rearrange("g c d -> c g d"), [], [bf("poolw_f")], bf("poolw_f"))
    dma("sp", pedge_s[:], pedge, [], [bf("pedge_s")], bf("pedge_s"))
    dma("sp", wr_f[:], w_router.rearrange("(dc p) e -> p dc e", p=128), [], [bf("wr_f")], bf("wr_f"))

    P.op("pool", lambda e: e.memset(ident_f[:], 0.0), writes=[bf("ident_f")])
    P.op("pool", lambda e: e.affine_select(out=ident_f[:], in_=ident_f[:], pattern=[[-1, 128]],
                                           compare_op=ALU.not_equal, fill=1.0, base=0,
                                           channel_multiplier=1),
         reads=[bf("ident_f")], writes=[bf("ident_f")])
    P.op("dve", lambda e: e.tensor_copy(out=ident[:], in_=ident_f[:]), reads=[bf("ident_f")], writes=[bf("ident")])
    for g, w in enumerate(POOL_WINDOWS):
        P.op("dve", lambda e, g=g, w=w: e.tensor_scalar(out=tapm[:, g, :], in0=ident_f[:], scalar1=1.0 / w,
                                                        scalar2=None, op0=ALU.mult),
             reads=[bf("ident_f")], writes=[bf("tapm")])
    P.op("pool", lambda e: e.memset(onesm[:], 1.0 / 512.0), writes=[bf("onesm")])
    P.op("pool", lambda e: e.memset(epsb[:], EPS), writes=[bf("epsb")])
    P.op("pool", lambda e: e.memset(ubuf[:], 0.0), writes=[bf("ub%d" % i) for i in range(5)])
    P.op("pool", lambda e: e.memset(zpbuf[:], 0.0), writes=[bf("zb%d" % i) for i in range(5)])
    P.op("dve", lambda e: e.tensor_copy(out=wr_b[:], in_=wr_f[:]), reads=[bf("wr_f")], writes=[bf("wr_b")])
    P.op("dve", lambda e: e.tensor_copy(out=poolw_b[:], in_=poolw_f[:]), reads=[bf("poolw_f")], writes=[bf("poolw_b")])
    for cc in range(4):
        for k in range(KS):
            eng = "dve" if (k % 2 == 0) else "pool"
            P.op(eng, lambda e, cc=cc, k=k: e.tensor_scalar(out=dconv[:, cc, k, :], in0=ident_f[:],
                                                           scalar1=convk_s[:, cc, k:k + 1], scalar2=None,
                                                           op0=ALU.mult),
                 reads=[bf("ident_f"), bf("convk_s")], writes=[bf("dconv")])
    stg = [xt[0], xt[1], xr[0], xr[1]]
    stgB = [bf("xt0"), bf("xt1"), bf("xr0"), bf("xr1")]
    si = 0
    for dc in range(8):
        for (c0, c1) in ((0, 1024), (1024, 1536)):
            s, sB = stg[si % 4], stgB[si % 4]
            si += 1
            n = c1 - c0
            dma("sp", s[:, 0:n], w_in[dc * 128:(dc + 1) * 128, c0:c1], [], [sB], sB)
            P.op("dve" if si % 2 else "pool",
                 lambda e, s=s, dc=dc, c0=c0, c1=c1, n=n: e.tensor_scalar(
                     out=win_b[:, dc, c0:c1], in0=s[:, 0:n], scalar1=pvec_s[:, 16 + dc:17 + dc],
                     scalar2=None, op0=ALU.mult),
                 reads=[sB, bf("pvec_s")], writes=[bf("win_b")])
    for dc in range(8):
        s, sB = stg[si % 4], stgB[si % 4]
        si += 1
        dma("sp", s[:], w_out[dc * 128:(dc + 1) * 128, :], [], [sB], sB)
        P.op("dve" if si % 2 else "pool", lambda e, s=s, dc=dc: e.tensor_copy(out=wout_b[:, dc, :], in_=s[:]),
             reads=[sB], writes=[bf("wout_b")])

    segs = []
    for s in range(NPSEQ):
        segs.append((xp[s * SEQ:(s + 1) * SEQ, :], 16, 0, 0, 16, s * SEQ))
    segs.append((xs, 18, 1, 1, 16, NPSEQ * SEQ))

    rot = {"ps": 0, "xt": 0, "xr": 0, "hb": 0, "sig": 0, "dt": 0, "pl": 0, "x1": 0, "st": 0}

    def nxt(key, n):
        v = rot[key]
        rot[key] = (v + 1) % n
        return v

    def psum_bank():
        i = nxt("ps", 6)
        return ps[i], psB[i]

    def stat_slot():
        i = nxt("st", 8)
        return stat[:, i, :], bf("stat%d" % i)

    def rsqrt_chain(src_ap, srcB, scale, out_ap, outB, tmp_ap, tmpB, eps_ap=None):
        P.op("act", lambda e: e.activation(out=tmp_ap, in_=src_ap, func=AF.Ln, bias=epsb[:, 0:1], scale=scale),
             reads=[srcB, bf("epsb")], writes=[tmpB])
        P.op("act", lambda e: e.activation(out=out_ap, in_=tmp_ap, func=AF.Exp, scale=-0.5), reads=[tmpB], writes=[outB])

    tile_counter = [0]

    for (src, ntile, stype, st0, nst, obase) in segs:
        L = ntile * 128
        nblk = (ntile + 3) // 4
        P.op("pool", lambda e, L=L: e.memset(ubuf[:, :, PADL + L:PADL + L + PADL], 0.0),
             reads=[], writes=[bf("ub4")])
        P.op("pool", lambda e, L=L: e.memset(zpbuf[:, :, PADL + L:PADL + L + PADL], 0.0),
             reads=[], writes=[bf("zb4")])
        blocks = []
        for b in range(nblk):
            t0 = b * 4
            nt_b = min(4, ntile - t0)
            blocks.append((b, t0, nt_b, nt_b * 128))

        for (b, t0, nt_b, nb) in blocks:
            hs = nxt("hb", 2)
            hTs, hTB = hT[hs], bf("hT%d" % hs)
            for ti in range(nt_b):
                t = t0 + ti
                xi = nxt("xt", 2)
                xtile, xB = xt[xi], bf("xt%d" % xi)
                dma("sp", xtile[:], src[t * 128:(t + 1) * 128, :], [], [xB], xB)
                st, stB = stat_slot()
                P.op("act", lambda e, xtile=xtile, st=st: e.activation(out=junk[:], in_=xtile[:], func=AF.Square,
                                                                      accum_out=st[:, 0:1]),
                     reads=[xB], writes=[bf("junk"), stB])
                rsqrt_chain(st[:, 0:1], stB, 1.0 / D, st[:, 2:3], stB, st[:, 1:2], stB)
                hi = ti % 2
                hbt, hbB = hb[hi], bf("hb%d" % hi)
                P.op("dve", lambda e, hbt=hbt, xtile=xtile, st=st: e.tensor_scalar(
                    out=hbt[:], in0=xtile[:], scalar1=st[:, 2:3], scalar2=None, op0=ALU.mult),
                     reads=[xB, stB], writes=[hbB])
                pi = nxt("ps", 6)
                pst, pstB = ps[pi], psB[pi]
                pv = pst[:].bitcast(BF16)
                for dc in range(8):
                    P.op("pe", lambda e, pv=pv, hbt=hbt, dc=dc: e.transpose(
                        out=pv[:, dc * 128:(dc + 1) * 128], in_=hbt[:, dc * 128:(dc + 1) * 128], identity=ident[:]),
                         reads=[hbB, bf("ident")], writes=[pstB])
                P.op("act", lambda e, pv=pv, hTs=hTs, ti=ti: e.activation(
                    out=hTs[:, :, ti * 128:(ti + 1) * 128], in_=pv.rearrange("p (a b) -> p a b", a=8),
                    func=AF.Copy),
                     reads=[pstB], writes=[hTB])
            c0 = PADL + t0 * 128
            for cc in range(4):
                pg, pgB = psum_bank()
                for dc in range(8):
                    P.op("pe", lambda e, pg=pg, dc=dc, cc=cc, hTs=hTs, nb=nb: e.matmul(
                        out=pg[:, 0:nb], lhsT=win_b[:, dc, 512 + cc * 128:512 + (cc + 1) * 128],
                        rhs=hTs[:, dc, 0:nb], start=(dc == 0), stop=(dc == 7)),
                         reads=[hTB, bf("win_b")], writes=[pgB])
                si_ = nxt("sig", 2)
                sg, sgB = sig[si_], bf("sig%d" % si_)
                P.op("act", lambda e, sg=sg, pg=pg, nb=nb: e.activation(out=sg[:, 0:nb], in_=pg[:, 0:nb],
                                                                        func=AF.Sigmoid),
                     reads=[pgB], writes=[sgB])
                pa, paB = psum_bank()
                for dc in range(8):
                    P.op("pe", lambda e, pa=pa, dc=dc, cc=cc, hTs=hTs, nb=nb: e.matmul(
                        out=pa[:, 0:nb], lhsT=win_b[:, dc, cc * 128:(cc + 1) * 128],
                        rhs=hTs[:, dc, 0:nb], start=(dc == 0), stop=(dc == 7)),
                         reads=[hTB, bf("win_b")], writes=[paB])
                P.op("dve", lambda e, pa=pa, sg=sg, cc=cc, c0=c0, nb=nb: e.tensor_tensor(
                    out=ubuf[:, cc, c0:c0 + nb], in0=pa[:, 0:nb], in1=sg[:, 0:nb], op=ALU.mult),
                     reads=[paB, sgB], writes=[bf("ub%d" % b)])
            for g in range(4):
                pz, pzB = psum_bank()
                for dc in range(8):
                    P.op("pe", lambda e, pz=pz, dc=dc, g=g, hTs=hTs, nb=nb: e.matmul(
                        out=pz[:, 0:nb], lhsT=win_b[:, dc, 1024 + g * 128:1024 + (g + 1) * 128],
                        rhs=hTs[:, dc, 0:nb], start=(dc == 0), stop=(dc == 7)),
                         reads=[hTB, bf("win_b")], writes=[pzB])
                P.op("act", lambda e, pz=pz, g=g, c0=c0, nb=nb: e.activation(
                    out=zpbuf[:, g, c0:c0 + nb], in_=pz[:, 0:nb], func=AF.Copy),
                     reads=[pzB], writes=[bf("zb%d" % b)])

        for (b, t0, nt_b, nb) in blocks:
            c0 = PADL + t0 * 128
            ubs = [bf("ub%d" % bb) for bb in (b - 1, b, b + 1) if 0 <= bb <= 4]
            zbs = [bf("zb%d" % bb) for bb in (b - 1, b, b + 1) if 0 <= bb <= 4]
            for cc in range(4):
                pc, pcB = psum_bank()
                for k in range(KS):
                    P.op("pe", lambda e, pc=pc, cc=cc, k=k, c0=c0, nb=nb: e.matmul(
                        out=pc[:, 0:nb], lhsT=dconv[:, cc, k, :],
                        rhs=ubuf[:, cc, c0 + k - 15:c0 + k - 15 + nb], start=(k == 0), stop=(k == KS - 1)),
                         reads=ubs + [bf("dconv")], writes=[pcB])
                P.op("act", lambda e, pc=pc, cc=cc, nb=nb: e.activation(
                    out=vb[:, cc, 0:nb], in_=pc[:, 0:nb], func=AF.Identity, bias=pvec_s[:, cc:cc + 1]),
                     reads=[pcB, bf("pvec_s")], writes=[bf("vb")])
                P.op("act", lambda e, pc=pc, cc=cc, nb=nb: e.activation(
                    out=sq[:, cc, 0:nb], in_=pc[:, 0:nb], func=AF.Square, bias=pvec_s[:, cc:cc + 1]),
                     reads=[pcB, bf("pvec_s")], writes=[bf("sq")])
            pm, pmB = psum_bank()
            for cc in range(4):
                P.op("pe", lambda e, pm=pm, cc=cc, nb=nb: e.matmul(out=pm[:, 0:nb], lhsT=onesm[:], rhs=vb[:, cc, 0:nb],
                                                                   start=(cc == 0), stop=(cc == 3)),
                     reads=[bf("vb"), bf("onesm")], writes=[pmB])
            pe2, pe2B = psum_bank()
            for cc in range(4):
                P.op("pe", lambda e, pe2=pe2, cc=cc, nb=nb: e.matmul(out=pe2[:, 0:nb], lhsT=onesm[:], rhs=sq[:, cc, 0:nb],
                                                                     start=(cc == 0), stop=(cc == 3)),
                     reads=[bf("sq"), bf("onesm")], writes=[pe2B])
            P.op("act", lambda e, pm=pm, nb=nb: e.activation(out=msq[:, 0:nb], in_=pm[:, 0:nb], func=AF.Square),
                 reads=[pmB], writes=[bf("msq")])
            P.op("dve", lambda e, pe2=pe2, nb=nb: e.tensor_tensor(out=lvar[:, 0:nb], in0=pe2[:, 0:nb], in1=msq[:, 0:nb],
                                                                  op=ALU.subtract),
                 reads=[pe2B, bf("msq")], writes=[bf("lvar")])
            P.op("act", lambda e, nb=nb: e.activation(out=lvar[:, 0:nb], in_=lvar[:, 0:nb], func=AF.Ln,
                                                      bias=epsb[:, 0:1], scale=1.0),
                 reads=[bf("lvar"), bf("epsb")], writes=[bf("lvar")])
            P.op("act", lambda e, nb=nb: e.activation(out=lrstd[:, 0:nb], in_=lvar[:, 0:nb], func=AF.Exp, scale=-0.5),
                 reads=[bf("lvar")], writes=[bf("lrstd")])
            for cc in range(4):
                di = nxt("dt", 2)
                dtp, dtB = dtmp[di], bf("dtmp%d" % di)
                P.op("dve", lambda e, dtp=dtp, cc=cc, pm=pm, nb=nb: e.tensor_tensor(
                    out=dtp[:, 0:nb], in0=vb[:, cc, 0:nb], in1=pm[:, 0:nb], op=ALU.subtract),
                     reads=[bf("vb"), pmB], writes=[dtB])
                P.op("dve", lambda e, dtp=dtp, nb=nb: e.tensor_tensor(
                    out=dtp[:, 0:nb], in0=dtp[:, 0:nb], in1=lrstd[:, 0:nb], op=ALU.mult),
                     reads=[dtB, bf("lrstd")], writes=[dtB])
                P.op("act", lambda e, dtp=dtp, cc=cc, nb=nb: e.activation(
                    out=ycT[:, cc, 0:nb], in_=dtp[:, 0:nb], func=AF.Silu,
                    bias=pvec_s[:, 8 + cc:9 + cc], scale=pvec_s[:, 4 + cc:5 + cc]),
                     reads=[dtB, bf("pvec_s")], writes=[bf("ycT")])
            for g, w in enumerate(POOL_WINDOWS):
                pp, ppB = psum_bank()
                for j in range(w):
                    off = c0 - w // 2 + j
                    P.op("pe", lambda e, pp=pp, g=g, off=off, j=j, w=w, nb=nb: e.matmul(
                        out=pp[:, 0:nb], lhsT=tapm[:, g, :], rhs=zpbuf[:, g, off:off + nb],
                        start=(j == 0), stop=(j == w - 1)),
                         reads=zbs + [bf("tapm")], writes=[ppB])
                pli = nxt("pl", 2)
                pl, plB = pooled[pli], bf("pooled%d" % pli)
                edges = []
                if stype == 0:
                    lo_pos, hi_pos = 0, L
                else:
                    lo_pos, hi_pos = 128, 128 + SEQ
                if t0 * 128 <= lo_pos < t0 * 128 + nb:
                    edges.append((0, lo_pos - t0 * 128))
                if t0 * 128 < hi_pos <= t0 * 128 + nb:
                    edges.append((1, hi_pos - 8 - t0 * 128))
                for (side, ec) in edges:
                    P.op("dve", lambda e, pp=pp, g=g, side=side, ec=ec, stype=stype: e.tensor_tensor(
                        out=pp[:, ec:ec + 8], in0=pp[:, ec:ec + 8], in1=pedge_s[:, stype, g, side, :], op=ALU.mult),
                         reads=[ppB, bf("pedge_s")], writes=[ppB])
                P.op("dve", lambda e, pp=pp, pl=pl, g=g, c0=c0, nb=nb: e.tensor_tensor(
                    out=pl[:, 0:nb], in0=pp[:, 0:nb], in1=zpbuf[:, g, c0:c0 + nb], op=ALU.subtract),
                     reads=[ppB, bf("zb%d" % b)], writes=[plB])
                pq, pqB = psum_bank()
                P.op("pe", lambda e, pq=pq, pl=pl, g=g, nb=nb: e.matmul(out=pq[:, 0:nb], lhsT=poolw_b[:, g, :],
                                                                         rhs=pl[:, 0:nb], start=True, stop=True),
                     reads=[plB, bf("poolw_b")], writes=[pqB])
                P.op("act", lambda e, pq=pq, g=g, nb=nb: e.activation(out=ypT[:, g, 0:nb], in_=pq[:, 0:nb], func=AF.Copy,
                                                                      scale=pvec_s[:, 12 + g:13 + g]),
                     reads=[pqB, bf("pvec_s")], writes=[bf("ypT")])
            for ti in range(nt_b):
                t = t0 + ti
                if not (st0 <= t < st0 + nst):
                    continue
                otok = obase + (t - st0) * 128
                gt = otok // 128
                xi = nxt("xr", 2)
                xrt, xrB = xr[xi], bf("xr%d" % xi)
                dma("sp", xrt[:], src[t * 128:(t + 1) * 128, :], [], [xrB], xrB)
                for half in range(2):
                    po, poB = ps[6 + half], psB[6 + half]
                    for kc in range(8):
                        lhs = ycT[:, kc, ti * 128:(ti + 1) * 128] if kc < 4 else ypT[:, kc - 4, ti * 128:(ti + 1) * 128]
                        P.op("pe", lambda e, po=po, lhs=lhs, kc=kc, half=half: e.matmul(
                            out=po[:, :], lhsT=lhs, rhs=wout_b[:, kc, half * 512:(half + 1) * 512],
                            start=(kc == 0), stop=(kc == 7)),
                             reads=[bf("ycT"), bf("ypT"), bf("wout_b")], writes=[poB])
                x1i = nxt("x1", 2)
                x1s, x1B = x1t[0], bf("x1t0")
                h2s, h2B = h2t[x1i], bf("h2t%d" % x1i)
                h2Ts, h2TB = h2T[x1i], bf("h2T%d" % x1i)
                for half in range(2):
                    P.op("dve", lambda e, half=half, x1s=x1s, xrt=xrt: e.tensor_tensor(
                        out=x1s[:, half * 512:(half + 1) * 512], in0=ps[6 + half][:, :],
                        in1=xrt[:, half * 512:(half + 1) * 512], op=ALU.add),
                         reads=[psB[6 + half], xrB], writes=[x1B])
                dma("pool", x1_o[otok:otok + 128, :], x1s[:], [x1B], [], x1B)
                st, stB = stat_slot()
                P.op("act", lambda e, x1s=x1s, st=st: e.activation(out=junk[:], in_=x1s[:], func=AF.Square,
                                                                  accum_out=st[:, 0:1]),
                     reads=[x1B], writes=[bf("junk"), stB])
                rsqrt_chain(st[:, 0:1], stB, 1.0 / D, st[:, 2:3], stB, st[:, 1:2], stB)
                P.op("dve", lambda e, h2s=h2s, x1s=x1s, st=st: e.scalar_tensor_tensor(
                    out=h2s[:], in0=x1s[:], scalar=st[:, 2:3], in1=g2b_s[:], op0=ALU.mult, op1=ALU.mult),
                     reads=[x1B, stB, bf("g2b_s")], writes=[h2B])
                dma("pool", h2_o[otok:otok + 128, :], h2s[:], [h2B], [], h2B)
                pi = nxt("ps", 6)
                pst, pstB = ps[pi], psB[pi]
                pv = pst[:].bitcast(BF16)
                for dc in range(8):
                    P.op("pe", lambda e, pv=pv, h2s=h2s, dc=dc: e.transpose(
                        out=pv[:, dc * 128:(dc + 1) * 128], in_=h2s[:, dc * 128:(dc + 1) * 128], identity=ident[:]),
                         reads=[h2B, bf("ident")], writes=[pstB])
                P.op("act", lambda e, pv=pv, h2Ts=h2Ts: e.activation(
                    out=h2Ts[:], in_=pv.rearrange("p (a b) -> p a b", a=8), func=AF.Copy),
                     reads=[pstB], writes=[h2TB])
                plg, plgB = psum_bank()
                for dc in range(8):
                    P.op("pe", lambda e, plg=plg, h2Ts=h2Ts, dc=dc: e.matmul(
                        out=plg[:, 0:NE], lhsT=h2Ts[:, dc, :], rhs=wr_b[:, dc, :], start=(dc == 0), stop=(dc == 7)),
                         reads=[h2TB, bf("wr_b")], writes=[plgB])
                P.op("dve", lambda e, plg=plg, st=st: e.tensor_reduce(out=st[:, 3:4], in_=plg[:, 0:NE], axis=AX.X,
                                                                      op=ALU.max, negate=True),
                     reads=[plgB], writes=[stB])
                ei = gt % 2
                P.op("act", lambda e, plg=plg, st=st, ei=ei: e.activation(
                    out=esm[:, ei, :], in_=plg[:, 0:NE], func=AF.Exp, bias=st[:, 3:4], scale=1.0,
                    accum_out=st[:, 4:5]),
                     reads=[plgB, stB], writes=[bf("esm%d" % ei), stB])
                P.op("dve", lambda e, st=st: e.reciprocal(out=st[:, 5:6], in_=st[:, 4:5]), reads=[stB], writes=[stB])
                P.op("dve", lambda e, st=st, ei=ei, gt=gt: e.tensor_scalar(
                    out=probs_s[:, gt, :], in0=esm[:, ei, :], scalar1=st[:, 5:6], scalar2=None, op0=ALU.mult),
                     reads=[bf("esm%d" % ei), stB], writes=[bf("probs_s")])
    dma("pool", pr_o, probs_s[:], [bf("probs_s")], [], bf("probs_s"))
    P.final_wait("pool", [bf("probs_s"), bf("x1t0"), bf("h2t0"), bf("h2t1")])
    P.emit()
    return nc, es


def _pool_edge_consts():
    pe = np.ones((4, 2, 8), np.float32)
    Lq = 64
    t = np.arange(Lq)
    for g, w in enumerate(POOL_WINDOWS):
        lo = np.maximum(t - w // 2, 0)
        hi = np.minimum(t + w // 2 - 1, Lq - 1)
        cnt = (hi - lo + 1).astype(np.float32)
        pe[g, 0, :] = w / cnt[:8]
        pe[g, 1, :] = w / cnt[-8:]
    return pe


def _phase_a_inputs(inputs, c):
    f = np.float32
    xp = np.ascontiguousarray(inputs["x_prompt"][4 * c:4 * c + 4]).reshape(NPSEQ * SEQ, D)
    sq, half = c // 2, c % 2
    xs_full = inputs["x_sample"][sq]
    xs = np.zeros((SEXT, D), f)
    lo = half * SEQ - 128
    a, b = max(lo, 0), min(lo + SEXT, 2 * SEQ)
    xs[a - lo:b - lo] = xs_full[a:b]
    pvec = np.zeros((128, 24), f)
    pvec[:, 0:4] = inputs["conv_b"][0].reshape(4, 128).T
    pvec[:, 4:8] = inputs["conv_ln_g"][0].reshape(4, 128).T
    pvec[:, 8:12] = inputs["conv_ln_b"][0].reshape(4, 128).T
    pvec[:, 12:16] = inputs["pool_scale"][0].T
    pvec[:, 16:24] = inputs["norm1_g"][0].reshape(8, 128).T
    convk = np.ascontiguousarray(inputs["conv_k"][0, :, 0, :].reshape(KS, 4, 128).transpose(2, 1, 0))
    pe = _pool_edge_consts()
    pedge = np.ones((2, 4, 2, 8), f)
    pedge[0] = pe
    if half == 0:
        pedge[1, :, 0, :] = pe[:, 0, :]
    else:
        pedge[1, :, 1, :] = pe[:, 1, :]
    return {
        "xp": xp, "xs": xs,
        "w_in": np.ascontiguousarray(inputs["w_in"][0]), "w_out": np.ascontiguousarray(inputs["w_out"][0]),
        "w_router": np.ascontiguousarray(inputs["w_router"][0]),
        "convk": convk, "pvec": pvec,
        "g2b": np.ascontiguousarray(np.broadcast_to(inputs["norm2_g"][0][None, :], (128, D))),
        "pool_w": np.ascontiguousarray(inputs["pool_w"][0]),
        "pedge": np.ascontiguousarray(np.broadcast_to(pedge[None], (128, 2, 4, 2, 8))),
    }


def run_phase_a(inputs, trace=False):
    nc, es = build_phase_a()
    in_maps = [_phase_a_inputs(inputs, c) for c in range(NCORES)]
    res = run_bass_kernel_spmd(nc, in_maps, core_ids=list(range(NCORES)))
    es.close()
    return res


NST = 11
NSLOT = NST * 128
NBIS = 24
DEX = 2048


def build_phase_b():
    nc = bass.Bass("TRN2", target_bir_lowering=False)
    es = ExitStack()
    P = Prog(nc, es)

    def din(name, shape, dt=F32):
        return nc.dram_tensor(name, list(shape), dt, kind="ExternalInput").ap()

    x1_i = din("x1", [TLOC, D])
    h2_i = din("h2", [TLOC, D], BF16)
    pa_i = din("pa", [128, NCORES, NT, NE])
    po_i = din("po", [128, NT, NE])
    potok_i = din("potok", [TLOC, NE])
    pp_i = din("pp", [TLOC, 256])
    wg_i = din("w_gate", [NE, D, DEX])
    wu_i = din("w_up", [NE, D, DEX])
    wd_i = din("w_down", [NE, DEX, D])
    wple_i = din("w_ple", [256, D])
    wpg_i = din("w_pg", [D, D])
    bpgb_i = din("bpgb", [128, D])
    fgb_i = din("fgb", [128, D])
    y_o = nc.dram_tensor("y", [TLOC, D], F32, kind="ExternalOutput").ap()
    x1scr = nc.dram_tensor("x1scr", [TLOC, D], F32, kind="Internal").ap()
    Gd = nc.dram_tensor("Gd", [NT * NE, 128], F32, kind="Internal").ap()

    B = {}

    def bf(name):
        if name not in B:
            B[name] = P.buf(name)
        return B[name]

    def dma(eng, out, in_, reads, writes, dbuf):
        return P.op(eng, lambda e: e.dma_start(out=out, in_=in_), reads=reads, writes=writes, dma=dbuf)

    rot = {}

    def nxt(key, n):
        v = rot.get(key, 0)
        rot[key] = (v + 1) % n
        return v

    ps = [es.enter_context(nc.psum_tensor("ps%d" % i, [128, 512], F32)) for i in range(8)]
    psB = [bf("ps%d" % i) for i in range(8)]

    def psum_bank():
        i = nxt("ps", 8)
        return ps[i], psB[i]

    sbp = lambda name, shape, dt: _sb(nc, es, name, shape, dt)
    ident_f = sbp("ident_f", [128, 128], F32)
    ident = sbp("ident", [128, 128], BF16)
    ones_f = sbp("ones_f", [128, 128], F32)
    ones_b = sbp("ones_b", [128, 128], BF16)
    epsb = sbp("epsb", [128, 1], F32)
    thr = sbp("thr", [128, 2 * NE], F32)
    C_b = sbp("C_b", [128, NE, NT], F32)
    siota = sbp("siota", [128, NST], F32)
    stat = sbp("stat", [128, 8, 8], F32)

    P.op("pool", lambda e: e.memset(ident_f[:], 0.0), writes=[bf("ident_f")])
    P.op("pool", lambda e: e.affine_select(out=ident_f[:], in_=ident_f[:], pattern=[[-1, 128]],
                                           compare_op=ALU.not_equal, fill=1.0, base=0, channel_multiplier=1),
         reads=[bf("ident_f")], writes=[bf("ident_f")])
    P.op("dve", lambda e: e.tensor_copy(out=ident[:], in_=ident_f[:]), reads=[bf("ident_f")], writes=[bf("ident")])
    P.op("pool", lambda e: e.memset(ones_f[:], 1.0), writes=[bf("ones_f")])
    P.op("pool", lambda e: e.memset(ones_b[:], 1.0), writes=[bf("ones_b")])
    P.op("pool", lambda e: e.memset(epsb[:], EPS), writes=[bf("epsb")])
    for i in range(8):
        r0 = i * (TLOC // 8)
        dma("sp", x1scr[r0:r0 + TLOC // 8, :], x1_i[r0:r0 + TLOC // 8, :], [], [bf("x1scr")], bf("x1cp"))

    with ExitStack() as es1:
        sb1 = lambda name, shape, dt: _sb(nc, es1, name, shape, dt)
        PA = sb1("PA", [128, NCORES, NT, NE], F32)
        PO = sb1("PO", [128, NT, NE], F32)
        cmpj = sb1("cmpj", [128, NCORES, 64], F32)
        cmpj2 = sb1("cmpj2", [128, NCORES, 64], F32)
        negmid = sb1("negmid", [128, 2 * NE], F32)
        cnt = sb1("cnt", [128, 2 * NE], F32)
        mid = sb1("mid", [128, 2 * NE], F32)
        capt = sb1("capt", [128, 2 * NE], F32)
        ge = sb1("ge", [128, 2 * NE], F32)
        mask_f = sb1("mask_f", [128, NT, NE], F32)
        mask_b = sb1("mask_b", [128, NT * NE], BF16)
        U_f = sb1("U_f", [128, 128], F32)
        U_b = sb1("U_b", [128, 128], BF16)
        tot_s = sb1("tot_s", [128, NT, NE], F32)
        cum_s = sb1("cum_s", [128, NT, NE], F32)
        ones80 = sb1("ones80", [128, NT], F32)
        Gs = sb1("Gs", [128, NT * NE], F32)
        GTs = sb1("GTs", [128, 10, 128], F32)
        si_i = sb1("si_i", [128, NST], I32)

        dma("sp", PA[:], pa_i, [], [bf("PA")], bf("PA"))
        dma("sp", PO[:], po_i, [], [bf("PO")], bf("PO"))
        P.op("pool", lambda e: e.memset(thr[:], 0.0), writes=[bf("thr")])
        P.op("pool", lambda e: e.memset(capt[:, 0:NE], float(2 * NCORES * 64 * 128 // NE)), writes=[bf("capt")])
        P.op("pool", lambda e: e.memset(ones80[:], 1.0), writes=[bf("ones80")])
        P.op("pool", lambda e: e.iota(si_i[:], pattern=[[128, NST]], base=0, channel_multiplier=1), writes=[bf("si_i")])
        P.op("dve", lambda e: e.tensor_copy(out=siota[:], in_=si_i[:]), reads=[bf("si_i")], writes=[bf("siota")])
        P.op("pool", lambda e: e.memset(U_f[:], 1.0), writes=[bf("U_f")])
        P.op("pool", lambda e: e.affine_select(out=U_f[:], in_=U_f[:], pattern=[[1, 128]], compare_op=ALU.is_ge,
                                               fill=0.0, base=0, channel_multiplier=-1),
             reads=[bf("U_f")], writes=[bf("U_f")])
        P.op("dve", lambda e: e.tensor_copy(out=U_b[:], in_=U_f[:]), reads=[bf("U_f")], writes=[bf("U_b")])

        groups = ((0, 0, 64), (1, 64, 80))
        NDVE = 12
        capv = (float(2 * NCORES * 64 * 128 // NE), float(2 * NCORES * 16 * 128 // NE))
        ntot = (float(NCORES * 64 * 128), float(NCORES * 16 * 128))
        P.op("pool", lambda e: e.memset(capt[:, NDVE:NE], 2 * capv[0] - ntot[0]), writes=[bf("capt")])
        P.op("pool", lambda e: e.memset(capt[:, NE:2 * NE], 2 * capv[1] - ntot[1]), writes=[bf("capt")])
        for it in range(NBIS):
            wdt = 2.0 ** (-(it + 1))
            P.op("dve", lambda e, wdt=wdt: e.tensor_scalar(out=mid[:], in0=thr[:], scalar1=wdt, scalar2=None, op0=ALU.add),
                 reads=[bf("thr")], writes=[bf("mid")])
            P.op("dve", lambda e, wdt=wdt: e.tensor_scalar(out=negmid[:], in0=thr[:], scalar1=-1.0, scalar2=-wdt,
                                                           op0=ALU.mult, op1=ALU.add),
                 reads=[bf("thr")], writes=[bf("negmid")])
            for (g, t0, t1) in groups:
                for ex in range(NE):
                    col = g * NE + ex
                    if g == 0 and ex < NDVE:
                        P.op("dve", lambda e, t0=t0, t1=t1, ex=ex, col=col: e.tensor_scalar(
                            out=cmpj[:, :, 0:t1 - t0], in0=PA[:, :, t0:t1, ex], scalar1=mid[:, col:col + 1], scalar2=None,
                            op0=ALU.is_ge, op1=ALU.add, accum_out=cnt[:, col:col + 1]),
                             reads=[bf("PA"), bf("mid")], writes=[bf("cmpj"), bf("cnt_d")], self_sync=False)
                    else:
                        P.op("act", lambda e, t0=t0, t1=t1, ex=ex, col=col: e.activation(
                            out=cmpj2[:, :, 0:t1 - t0], in_=PA[:, :, t0:t1, ex], func=AF.Sign,
                            bias=negmid[:, col:col + 1], scale=1.0, accum_out=cnt[:, col:col + 1]),
                             reads=[bf("PA"), bf("negmid")], writes=[bf("cmpj2"), bf("cnt_a")], self_sync=False)
            pt, ptB = psum_bank()
            P.op("pe", lambda e, pt=pt: e.matmul(out=pt[:, 0:2 * NE], lhsT=ones_f[:], rhs=cnt[:], start=True, stop=True),
                 reads=[bf("cnt_d"), bf("cnt_a"), bf("ones_f")], writes=[ptB])
            P.op("dve", lambda e, pt=pt: e.tensor_tensor(out=ge[:], in0=pt[:, 0:2 * NE], in1=capt[:], op=ALU.is_ge),
                 reads=[ptB, bf("capt")], writes=[bf("ge")])
            P.op("dve", lambda e, wdt=wdt: e.scalar_tensor_tensor(out=thr[:], in0=ge[:], scalar=wdt, in1=thr[:],
                                                                  op0=ALU.mult, op1=ALU.add),
                 reads=[bf("ge"), bf("thr")], writes=[bf("thr")])
        for (g, t0, t1) in groups:
            P.op("dve", lambda e, g=g, t0=t0, t1=t1: e.tensor_tensor(
                out=mask_f[:, t0:t1, :], in0=PO[:, t0:t1, :],
                in1=thr[:, g * NE:(g + 1) * NE].unsqueeze(1).broadcast_to([128, t1 - t0, NE]), op=ALU.is_ge),
                 reads=[bf("PO"), bf("thr")], writes=[bf("mask_f")])
        P.op("dve", lambda e: e.tensor_copy(out=mask_b[:], in_=mask_f[:].rearrange("p t e -> p (t e)")),
             reads=[bf("mask_f")], writes=[bf("mask_b")])
        chunks = ((0, 512), (512, 1024), (1024, 1280))
        for (a0, a1) in chunks:
            pc, pcB = psum_bank()
            P.op("pe", lambda e, pc=pc, a0=a0, a1=a1: e.matmul(out=pc[:, 0:a1 - a0], lhsT=U_b[:], rhs=mask_b[:, a0:a1],
                                                              start=True, stop=True),
                 reads=[bf("U_b"), bf("mask_b")], writes=[pcB])
            P.op("act", lambda e, pc=pc, a0=a0, a1=a1: e.activation(
                out=cum_s[:].rearrange("p t e -> p (t e)")[:, a0:a1], in_=pc[:, 0:a1 - a0], func=AF.Copy),
                 reads=[pcB], writes=[bf("cum_s")])
            pt, ptB = psum_bank()
            P.op("pe", lambda e, pt=pt, a0=a0, a1=a1: e.matmul(out=pt[:, 0:a1 - a0], lhsT=ones_b[:], rhs=mask_b[:, a0:a1],
                                                              start=True, stop=True),
                 reads=[bf("ones_b"), bf("mask_b")], writes=[ptB])
            P.op("act", lambda e, pt=pt, a0=a0, a1=a1: e.activation(
                out=tot_s[:].rearrange("p t e -> p (t e)")[:, a0:a1], in_=pt[:, 0:a1 - a0], func=AF.Copy),
                 reads=[ptB], writes=[bf("tot_s")])
        for ex in range(NE):
            P.op("dve", lambda e, ex=ex: e.tensor_tensor_scan(out=C_b[:, ex, :], data0=ones80[:], data1=tot_s[:, :, ex],
                                                              initial=0.0, op0=ALU.mult, op1=ALU.add),
                 reads=[bf("ones80"), bf("tot_s")], writes=[bf("C_b")])
        Gv = Gs[:].rearrange("p (t e) -> p t e", e=NE)
        P.op("dve", lambda e: e.tensor_tensor(out=Gv, in0=C_b[:].rearrange("p e t -> p t e"), in1=tot_s[:], op=ALU.subtract),
             reads=[bf("C_b"), bf("tot_s")], writes=[bf("Gs")])
        P.op("dve", lambda e: e.tensor_tensor(out=Gv, in0=Gv, in1=cum_s[:], op=ALU.add),
             reads=[bf("Gs"), bf("cum_s")], writes=[bf("Gs")])
        for k in range(10):
            pg, pgB = psum_bank()
            P.op("pe", lambda e, pg=pg, k=k: e.transpose(out=pg[:, 0:128], in_=Gs[:, k * 128:(k + 1) * 128], identity=ident_f[:]),
                 reads=[bf("Gs"), bf("ident_f")], writes=[pgB])
            P.op("act", lambda e, pg=pg, k=k: e.activation(out=GTs[:, k, :], in_=pg[:, 0:128], func=AF.Copy),
                 reads=[pgB], writes=[bf("GTs")])
        dma("sp", Gd.rearrange("(k p) c -> p k c", p=128), GTs[:], [bf("GTs")], [bf("Gd")], bf("GTs"))
        P.emit()
    P.barrier()

    with ExitStack() as es2:
        sb2 = lambda name, shape, dt: _sb(nc, es2, name, shape, dt)
        xeT = sb2("xeT", [128, 8, NSLOT], BF16)
        hT = sb2("hT", [128, 16, NSLOT], BF16)
        XE = [sb2("XE%d" % i, [128, D], BF16) for i in range(6)]
        wgu = [sb2("wgu%d" % i, [128, 2, 8, 512], BF16) for i in range(2)]
        wd_b = sb2("wd_b", [128, 16, D], BF16)
        stg = [sb2("stg%d" % i, [128, 2048], F32) for i in range(3)]
        ysc = [sb2("ysc%d" % i, [128, D], F32) for i in range(2)]
        sa = [sb2("sa%d" % i, [128, 512], BF16) for i in range(2)]
        cmp1 = sb2("cmp1", [128, NST, NT], F32)
        cmp2 = sb2("cmp2", [128, NST, 128], F32)
        Grow = sb2("Grow", [128, NST, 128], F32)
        jf = sb2("jf", [128, NST], F32)
        pf = sb2("pf", [128, NST], F32)
        rf = sb2("rf", [128, NST], F32)
        ridx = sb2("ridx", [128, NST], I32)
        nidx = [sb2("nidx%d" % i, [128, NST], I32) for i in range(2)]
        GP = [sb2("GP%d" % i, [128, NST, NE], F32) for i in range(2)]

        P.op("pool", lambda e: e.memset(Grow[:], 0.0), writes=[bf("Grow")])
        for i in range(6):
            P.op("pool", lambda e, i=i: e.memset(XE[i][:], 0.0), writes=[bf("XE%d" % i)])
        for i in range(2):
            P.op("pool", lambda e, i=i: e.memset(GP[i][:], 0.0), writes=[bf("GP%d" % i)])

        cast_rr = [0]

        def cast(out_ap, in_ap, reads, writes):
            k = cast_rr[0] % 2 + 1
            cast_rr[0] += 1
            if k == 1:
                P.op("act", lambda e: e.activation(out=out_ap, in_=in_ap, func=AF.Copy), reads=reads, writes=writes)
            else:
                P.op("dve", lambda e: e.tensor_copy(out=out_ap, in_=in_ap), reads=reads, writes=writes)

        def load_gu(ex, fg):
            slot = (ex * 4 + fg) % 2
            wB = bf("wgu%d" % slot)
            for m, wsrc in enumerate((wg_i, wu_i)):
                v = wsrc[ex].rearrange("(dc p) f -> p dc f", p=128)
                for h in range(2):
                    si = nxt("stg", 3)
                    sB = bf("stg%d" % si)
                    dma("sp", stg[si][:].rearrange("p (a b) -> p a b", a=4), v[:, h * 4:(h + 1) * 4, fg * 512:(fg + 1) * 512],
                        [], [sB], sB)
                    cast(wgu[slot][:, m, h * 4:(h + 1) * 4, :], stg[si][:].rearrange("p (a b) -> p a b", a=4), [sB], [wB])

        def load_wd(ex, fg):
            vd = wd_i[ex].rearrange("(fc p) d -> p fc d", p=128)
            for h in range(2):
                si = nxt("stg", 3)
                sB = bf("stg%d" % si)
                fc0 = fg * 4 + h * 2
                dma("sp", stg[si][:].rearrange("p (a b) -> p a b", a=2), vd[:, fc0:fc0 + 2, :], [], [sB], sB)
                cast(wd_b[:, fc0:fc0 + 2, :], stg[si][:].rearrange("p (a b) -> p a b", a=2), [sB], [bf("wd%d" % fg)])

        NXE = 6

        def prep_idx(ex):
            par = ex % 2
            nB = bf("nidx%d" % par)
            gB = bf("GP%d" % par)
            P.op("dve", lambda e: e.tensor_tensor(out=cmp1[:], in0=C_b[:, ex:ex + 1, :].broadcast_to([128, NST, NT]),
                                                  in1=siota[:, :].unsqueeze(2).broadcast_to([128, NST, NT]), op=ALU.is_le),
                 reads=[bf("C_b"), bf("siota")], writes=[bf("cmp1")])
            P.op("dve", lambda e: e.tensor_reduce(out=jf[:], in_=cmp1[:], axis=AX.X, op=ALU.add),
                 reads=[bf("cmp1")], writes=[bf("jf")])
            P.op("dve", lambda e: e.tensor_scalar(out=rf[:], in0=jf[:], scalar1=float(NE), scalar2=float(ex),
                                                  op0=ALU.mult, op1=ALU.add),
                 reads=[bf("jf")], writes=[bf("rf")])
            P.op("dve", lambda e: e.tensor_copy(out=ridx[:], in_=rf[:]), reads=[bf("rf")], writes=[bf("ridx")])
            P.op("pool", lambda e: e.memset(Grow[:, 0, 0:1], 0.0), reads=[], writes=[bf("Grow")])
            for st in range(NST):
                P.op("pool", lambda e, st=st: e.indirect_dma_start(
                    out=Grow[:, st, :], out_offset=None, in_=Gd[:, :],
                    in_offset=bass.IndirectOffsetOnAxis(ap=ridx[:, st:st + 1], axis=0),
                    bounds_check=P.reg(e, NT * NE - 1), oob_is_err=False),
                     reads=[bf("ridx"), bf("Gd")], writes=[bf("Grow")], dma=bf("Grow"), skip_waw=(st > 0))
            P.op("dve", lambda e: e.tensor_tensor(out=cmp2[:], in0=Grow[:],
                                                  in1=siota[:, :].unsqueeze(2).broadcast_to([128, NST, 128]), op=ALU.is_le),
                 reads=[bf("Grow"), bf("siota")], writes=[bf("cmp2")])
            P.op("dve", lambda e: e.tensor_reduce(out=pf[:], in_=cmp2[:], axis=AX.X, op=ALU.add),
                 reads=[bf("cmp2")], writes=[bf("pf")])
            P.op("dve", lambda e: e.scalar_tensor_tensor(out=rf[:], in0=jf[:], scalar=128.0, in1=pf[:],
                                                         op0=ALU.mult, op1=ALU.add),
                 reads=[bf("jf"), bf("pf")], writes=[bf("rf")])
            P.op("dve", lambda e: e.tensor_copy(out=nidx[par][:], in_=rf[:]), reads=[bf("rf")], writes=[nB])
            P.op("pool", lambda e: e.memset(GP[par][:, 0, 0:1], 0.0), reads=[], writes=[gB])
            for st in range(NST):
                P.op("pool", lambda e, st=st: e.indirect_dma_start(
                    out=GP[par][:, st, :], out_offset=None, in_=potok_i[:, :],
                    in_offset=bass.IndirectOffsetOnAxis(ap=nidx[par][:, st:st + 1], axis=0),
                    bounds_check=P.reg(e, TLOC - 1), oob_is_err=False),
                     reads=[nB], writes=[gB], dma=gB, skip_waw=(st > 0))

        def prep_x(ex):
            par = ex % 2
            nB = bf("nidx%d" % par)
            for st in range(NST):
                xi = nxt("XE", NXE)
                xB = bf("XE%d" % xi)
                P.op("pool", lambda e, st=st, xi=xi: e.indirect_dma_start(
                    out=XE[xi][:, :], out_offset=None, in_=h2_i[:, :],
                    in_offset=bass.IndirectOffsetOnAxis(ap=nidx[par][:, st:st + 1], axis=0),
                    bounds_check=P.reg(e, TLOC - 1), oob_is_err=False),
                     reads=[nB], writes=[xB], dma=xB)
                pt, ptB = psum_bank()
                pv = pt[:].bitcast(BF16)
                for dc in range(8):
                    P.op("pe", lambda e, pv=pv, xi=xi, dc=dc: e.transpose(
                        out=pv[:, dc * 128:(dc + 1) * 128], in_=XE[xi][:, dc * 128:(dc + 1) * 128], identity=ident[:]),
                         reads=[xB, bf("ident")], writes=[ptB])
                if st % 2:
                    P.op("dve", lambda e, pv=pv, st=st: e.tensor_copy(
                        out=xeT[:, :, st * 128:(st + 1) * 128], in_=pv.rearrange("p (a b) -> p a b", a=8)),
                         reads=[ptB], writes=[bf("xeT")])
                else:
                    P.op("act", lambda e, pv=pv, st=st: e.activation(
                        out=xeT[:, :, st * 128:(st + 1) * 128], in_=pv.rearrange("p (a b) -> p a b", a=8), func=AF.Copy),
                         reads=[ptB], writes=[bf("xeT")])

        nblks = ((0, 512), (512, 1024), (1024, NSLOT))

        def gateup(ex):
            for fg in range(4):
                if fg < 3:
                    load_gu(ex, fg + 1)
                elif ex + 1 < NE:
                    load_gu(ex + 1, 0)
                load_wd(ex, fg)
                slot = (ex * 4 + fg) % 2
                wB = bf("wgu%d" % slot)
                for fcl in range(4):
                    fc = fg * 4 + fcl
                    for (n0, n1) in nblks:
                        n = n1 - n0
                        pa, paB = psum_bank()
                        for dc in range(8):
                            P.op("pe", lambda e, pa=pa, dc=dc, fcl=fcl, slot=slot, n0=n0, n1=n1, n=n: e.matmul(
                                out=pa[:, 0:n], lhsT=wgu[slot][:, 0, dc, fcl * 128:(fcl + 1) * 128], rhs=xeT[:, dc, n0:n1],
                                start=(dc == 0), stop=(dc == 7)),
                                 reads=[wB, bf("xeT")], writes=[paB])
                        pb, pbB = psum_bank()
                        for dc in range(8):
                            P.op("pe", lambda e, pb=pb, dc=dc, fcl=fcl, slot=slot, n0=n0, n1=n1, n=n: e.matmul(
                                out=pb[:, 0:n], lhsT=wgu[slot][:, 1, dc, fcl * 128:(fcl + 1) * 128], rhs=xeT[:, dc, n0:n1],
                                start=(dc == 0), stop=(dc == 7)),
                                 reads=[wB, bf("xeT")], writes=[pbB])
                        si = nxt("sa", 2)
                        sB = bf("sa%d" % si)
                        P.op("act", lambda e, pa=pa, si=si, n=n: e.activation(out=sa[si][:, 0:n], in_=pa[:, 0:n], func=AF.Silu),
                             reads=[paB], writes=[sB])
                        P.op("dve", lambda e, pb=pb, si=si, fc=fc, n0=n0, n1=n1, n=n: e.tensor_tensor(
                            out=hT[:, fc, n0:n1], in0=pb[:, 0:n], in1=sa[si][:, 0:n], op=ALU.mult),
                             reads=[pbB, sB], writes=[bf("hT")])

        def down(ex):
            par = ex % 2
            nB = bf("nidx%d" % par)
            gB = bf("GP%d" % par)
            wdBs = [bf("wd%d" % fg) for fg in range(4)]
            for st in range(NST):
                yi = nxt("ysc", 2)
                yB = bf("ysc%d" % yi)
                for half in range(2):
                    py, pyB = psum_bank()
                    for fc in range(16):
                        P.op("pe", lambda e, py=py, fc=fc, st=st, half=half: e.matmul(
                            out=py[:, :], lhsT=hT[:, fc, st * 128:(st + 1) * 128], rhs=wd_b[:, fc, half * 512:(half + 1) * 512],
                            start=(fc == 0), stop=(fc == 15)),
                             reads=[bf("hT")] + wdBs, writes=[pyB])
                    P.op("act", lambda e, py=py, yi=yi, half=half, st=st: e.activation(
                        out=ysc[yi][:, half * 512:(half + 1) * 512], in_=py[:, :], func=AF.Copy,
                        scale=GP[par][:, st, ex:ex + 1]),
                         reads=[pyB, gB], writes=[yB])
                P.op("pool", lambda e, yi=yi, st=st: e.indirect_dma_start(
                    out=x1scr[:, :], out_offset=bass.IndirectOffsetOnAxis(ap=nidx[par][:, st:st + 1], axis=0),
                    in_=ysc[yi][:, :], in_offset=None, bounds_check=P.reg(e, TLOC - 1), oob_is_err=False, compute_op=ALU.add),
                     reads=[yB, nB, bf("x1scr")], writes=[bf("x1scr")], dma=bf("x1acc"))
                bf("ysc%d" % yi).readers[bf("x1acc")] = bf("x1acc").cnt

        prep_idx(0)
        prep_x(0)
        load_gu(0, 0)
        for ex in range(NE):
            if ex + 1 < NE:
                prep_idx(ex + 1)
            gateup(ex)
            if ex + 1 < NE:
                prep_x(ex + 1)
            down(ex)
        P.emit()
    P.barrier()

    with ExitStack() as es3:
        sb3 = lambda name, shape, dt: _sb(nc, es3, name, shape, dt)
        wpg_b = sb3("wpg_b", [128, 8, D], BF16)
        wple_b = sb3("wple_b", [128, 2, D], BF16)
        bpgb = sb3("bpgb_s", [128, D], F32)
        fgb = sb3("fgb_s", [128, D], F32)
        stg3 = [sb3("stg3_%d" % i, [128, D], F32) for i in range(2)]
        NS3 = 4
        x2 = [sb3("x2_%d" % i, [128, D], F32) for i in range(NS3)]
        pt_ = [sb3("pt_%d" % i, [128, 256], F32) for i in range(NS3)]
        x2b = [sb3("x2b%d" % i, [128, D], BF16) for i in range(NS3)]
        pb_ = [sb3("pb_%d" % i, [128, 256], BF16) for i in range(NS3)]
        x2T = [sb3("x2T%d" % i, [128, 8, 128], BF16) for i in range(NS3)]
        pT = [sb3("pT%d" % i, [128, 2, 128], BF16) for i in range(NS3)]
        gpre = [sb3("gpre%d" % i, [128, D], F32) for i in range(2)]
        x3 = [sb3("x3_%d" % i, [128, D], F32) for i in range(8)]
        yo = [sb3("yo%d" % i, [128, D], F32) for i in range(3)]
        junk = sb3("junk3", [128, D], BF16)
        gst = [sb3("gst%d" % i, [128, 8], F32) for i in range(2)]

        dma("sp", bpgb[:], bpgb_i, [], [bf("bpgb")], bf("bpgb"))
        dma("sp", fgb[:], fgb_i, [], [bf("fgb")], bf("fgb"))
        for dc in range(8):
            si = dc % 2
            sB = bf("stg3_%d" % si)
            dma("sp", stg3[si][:], wpg_i[dc * 128:(dc + 1) * 128, :], [], [sB], sB)
            P.op("dve" if dc % 2 else "pool", lambda e, si=si, dc=dc: e.tensor_copy(out=wpg_b[:, dc, :], in_=stg3[si][:]),
                 reads=[sB], writes=[bf("wpg_b")])
        for kc in range(2):
            si = kc % 2
            sB = bf("stg3_%d" % si)
            dma("sp", stg3[si][:], wple_i[kc * 128:(kc + 1) * 128, :], [], [sB], sB)
            P.op("dve" if kc % 2 else "pool", lambda e, si=si, kc=kc: e.tensor_copy(out=wple_b[:, kc, :], in_=stg3[si][:]),
                 reads=[sB], writes=[bf("wple_b")])

        def st_A(t):
            i = t % NS3
            x2B, ptB_, x2bB, pbB_ = bf("x2_%d" % i), bf("pt_%d" % i), bf("x2b%d" % i), bf("pb_%d" % i)
            dma("sp", x2[i][:], x1scr[t * 128:(t + 1) * 128, :], [bf("x1scr")], [x2B], x2B)
            dma("sp", pt_[i][:], pp_i[t * 128:(t + 1) * 128, :], [], [ptB_], ptB_)
            P.op("pool", lambda e, i=i: e.tensor_copy(out=x2b[i][:], in_=x2[i][:]), reads=[x2B], writes=[x2bB])
            P.op("pool", lambda e, i=i: e.tensor_copy(out=pb_[i][:], in_=pt_[i][:]), reads=[ptB_], writes=[pbB_])

        def st_B(t):
            i = t % NS3
            x2bB, pbB_, x2TB, pTB = bf("x2b%d" % i), bf("pb_%d" % i), bf("x2T%d" % i), bf("pT%d" % i)
            pa, paB = psum_bank()
            pv = pa[:].bitcast(BF16)
            for dc in range(8):
                P.op("pe", lambda e, pv=pv, i=i, dc=dc: e.transpose(
                    out=pv[:, dc * 128:(dc + 1) * 128], in_=x2b[i][:, dc * 128:(dc + 1) * 128], identity=ident[:]),
                     reads=[x2bB, bf("ident")], writes=[paB])
            P.op("act", lambda e, pv=pv, i=i: e.activation(out=x2T[i][:], in_=pv.rearrange("p (a b) -> p a b", a=8), func=AF.Copy),
                 reads=[paB], writes=[x2TB])
            pq, pqB = psum_bank()
            pv2 = pq[:].bitcast(BF16)
            for kc in range(2):
                P.op("pe", lambda e, pv2=pv2, i=i, kc=kc: e.transpose(
                    out=pv2[:, kc * 128:(kc + 1) * 128], in_=pb_[i][:, kc * 128:(kc + 1) * 128], identity=ident[:]),
                     reads=[pbB_, bf("ident")], writes=[pqB])
            P.op("dve", lambda e, pv2=pv2, i=i: e.tensor_copy(
                out=pT[i][:], in_=pv2[:, 0:256].rearrange("p (a b) -> p a b", a=2)),
                 reads=[pqB], writes=[pTB])

        def st_CD(t):
            i = t % NS3
            grp, ti = t // 4, t % 4
            par = grp % 2
            j = par * 4 + ti
            gsB = bf("gst%d" % par)
            x2B, x2TB, pTB, gB_, x3B = bf("x2_%d" % i), bf("x2T%d" % i), bf("pT%d" % i), bf("gpre%d" % (t % 2)), bf("x3_%d" % j)
            gp = gpre[t % 2]
            pgs, pes = [], []
            for half in range(2):
                pg, pgB = psum_bank()
                for kc in range(8):
                    P.op("pe", lambda e, pg=pg, i=i, kc=kc, half=half: e.matmul(
                        out=pg[:, :], lhsT=x2T[i][:, kc, :], rhs=wpg_b[:, kc, half * 512:(half + 1) * 512],
                        start=(kc == 0), stop=(kc == 7)),
                         reads=[x2TB, bf("wpg_b")], writes=[pgB])
                pgs.append((pg, pgB))
                pe_, peB = psum_bank()
                for kc in range(2):
                    P.op("pe", lambda e, pe_=pe_, i=i, kc=kc, half=half: e.matmul(
                        out=pe_[:, :], lhsT=pT[i][:, kc, :], rhs=wple_b[:, kc, half * 512:(half + 1) * 512],
                        start=(kc == 0), stop=(kc == 1)),
                         reads=[pTB, bf("wple_b")], writes=[peB])
                pes.append((pe_, peB))
            for half in range(2):
                hs = slice(half * 512, (half + 1) * 512)
                pg, pgB = pgs[half]
                pe_, peB = pes[half]
                P.op("dve", lambda e, pg=pg, gp=gp, hs=hs: e.tensor_tensor(out=gp[:, hs], in0=pg[:, :], in1=bpgb[:, hs], op=ALU.add),
                     reads=[pgB, bf("bpgb")], writes=[gB_])
                P.op("act", lambda e, gp=gp, hs=hs: e.activation(out=gp[:, hs], in_=gp[:, hs], func=AF.Sigmoid),
                     reads=[gB_], writes=[gB_])
                P.op("dve", lambda e, pe_=pe_, gp=gp, hs=hs: e.tensor_tensor(out=gp[:, hs], in0=pe_[:, :], in1=gp[:, hs], op=ALU.mult),
                     reads=[peB, gB_], writes=[gB_])
            P.op("pool", lambda e, gp=gp, i=i, j=j: e.tensor_tensor(out=x3[j][:], in0=gp[:], in1=x2[i][:], op=ALU.add),
                 reads=[gB_, x2B], writes=[x3B])
            P.op("act", lambda e, j=j, par=par, ti=ti: e.activation(out=junk[:], in_=x3[j][:], func=AF.Square,
                                                                    accum_out=gst[par][:, ti:ti + 1]),
                 reads=[x3B], writes=[bf("junk3"), gsB])

        def st_E(grp):
            par = grp % 2
            gsB = bf("gst%d" % par)
            P.op("act", lambda e, par=par: e.activation(out=gst[par][:, 4:8], in_=gst[par][:, 0:4], func=AF.Ln,
                                                        bias=epsb[:, 0:1], scale=1.0 / D),
                 reads=[gsB, bf("epsb")], writes=[gsB])
            P.op("act", lambda e, par=par: e.activation(out=gst[par][:, 4:8], in_=gst[par][:, 4:8], func=AF.Exp, scale=-0.5),
                 reads=[gsB], writes=[gsB])
            for ti in range(4):
                t = grp * 4 + ti
                k = t % 3
                j = par * 4 + ti
                yoB = bf("yo%d" % k)
                P.op("dve", lambda e, k=k, j=j, par=par, ti=ti: e.scalar_tensor_tensor(
                    out=yo[k][:], in0=x3[j][:], scalar=gst[par][:, 4 + ti:5 + ti], in1=fgb[:], op0=ALU.mult, op1=ALU.mult),
                     reads=[bf("x3_%d" % j), gsB, bf("fgb")], writes=[yoB])
                dma("pool", y_o[t * 128:(t + 1) * 128, :], yo[k][:], [yoB], [], yoB)

        st_A(0)
        st_A(1)
        st_B(0)
        for t in range(NT):
            if t + 2 < NT:
                st_A(t + 2)
            if t + 1 < NT:
                st_B(t + 1)
            st_CD(t)
            if t % 4 == 3:
                st_E(t // 4)
        P.final_wait("pool", [bf("yo0"), bf("yo1"), bf("yo2")])
        P.emit()
    return nc, es


def _phase_b_inputs(inputs, res1, c):
    f = np.float32
    probs = [np.asarray(res1[k]["probs"], dtype=f) for k in range(NCORES)]
    pa = np.ascontiguousarray(np.stack(probs, 0).transpose(1, 0, 2, 3))
    potok = np.ascontiguousarray(probs[c].transpose(1, 0, 2).reshape(TLOC, NE))
    sq, half = c // 2, c % 2
    pp = np.concatenate([inputs["p_prompt"][0, 4 * c:4 * c + 4].reshape(NPSEQ * SEQ, 256),
                         inputs["p_sample"][0, sq, half * SEQ:(half + 1) * SEQ]], 0)
    return {
        "x1": np.asarray(res1[c]["x1"]), "h2": np.asarray(res1[c]["h2"]),
        "pa": pa, "po": probs[c], "potok": potok, "pp": np.ascontiguousarray(pp),
        "w_gate": inputs["w_gate"][0], "w_up": inputs["w_up"][0], "w_down": inputs["w_down"][0],
        "w_ple": inputs["w_ple"][0], "w_pg": inputs["w_pg"][0],
        "bpgb": np.ascontiguousarray(np.broadcast_to(inputs["b_pg"][0][None, :], (128, D))),
        "fgb": np.ascontiguousarray(np.broadcast_to(inputs["final_g"][None, :], (128, D))),
    }


def kernel(**inputs):
    inputs = {k: np.asarray(v) for k, v in inputs.items()}
    nc1, es1 = build_phase_a()
    in1 = [_phase_a_inputs(inputs, c) for c in range(NCORES)]
    res1 = run_bass_kernel_spmd(nc1, in1, core_ids=list(range(NCORES))).results
    es1.close()
    nc2, es2 = build_phase_b()
    in2 = [_phase_b_inputs(inputs, res1, c) for c in range(NCORES)]
    res2 = run_bass_kernel_spmd(nc2, in2, core_ids=list(range(NCORES))).results
    es2.close()
    y_prompt = np.zeros((32, SEQ, D), np.float32)
    y_sample = np.zeros((4, 2 * SEQ, D), np.float32)
    for c in range(NCORES):
        y = np.asarray(res2[c]["y"], dtype=np.float32)
        y_prompt[4 * c:4 * c + 4] = y[:NPSEQ * SEQ].reshape(NPSEQ, SEQ, D)
        y_sample[c // 2, (c % 2) * SEQ:(c % 2 + 1) * SEQ] = y[NPSEQ * SEQ:]
    return (y_prompt, y_sample)
```
